# Optimizing a Trainium2 kernel written in Bass

```python
import math
import jax
import jax.numpy as jnp
from jax import lax
import numpy as np

D_MODEL = 2048
BATCH = 2
SEQ = 4096
DEPTH = 1

GRID_W = 64
N_ATTN_HEADS = 8
ATTN_WIDTH = D_MODEL // 2
HEAD_DIM = ATTN_WIDTH // N_ATTN_HEADS
HYENA_WIDTH = D_MODEL - ATTN_WIDTH
N_HYENA_GROUPS = 8
HYENA_ORDER = 2
N_DIRS = 2
SHORT_CONV_W = 3
FILTER_EMB = 33
FILTER_HIDDEN = 64
DECAY_TARGET = 1e-2
FAST_DECAY_PCT = 0.3
SLOW_DECAY_PCT = 1.5
WIN_ROWS_MAX = 8
WIN_COLS = 16
N_EXPERTS = 16
EC_CAPACITY_FACTOR = 2
EXPERT_FF = 1024
IN_WIDTH = 3 * ATTN_WIDTH + (HYENA_ORDER + 1) * HYENA_WIDTH
RMS_EPS = 1e-6

kernel_name = 'hybrid_na2d_hyena_ecmoe_block'


def rms_norm(x, g):
    xf = x.astype(jnp.float32)
    y = xf * lax.rsqrt(jnp.mean(xf * xf, axis=-1, keepdims=True) + RMS_EPS)
    return (y * g.astype(jnp.float32)).astype(x.dtype)


def group_rms_norm(y, g, n_groups):
    b, s, w = y.shape
    yg = y.reshape(b, s, n_groups, w // n_groups)
    return rms_norm(yg, g.reshape(n_groups, w // n_groups)).reshape(b, s, w)


def neighbourhood_attention(q, k, v, rpb):
    b, s, h, hd = q.shape
    rows = s // GRID_W
    win_r = min(WIN_ROWS_MAX, rows)
    r = jnp.arange(rows)
    c = jnp.arange(GRID_W)
    key_rows = jnp.clip(r - win_r // 2, 0, rows - win_r)[:, None] + jnp.arange(win_r)[None, :]
    c0 = jnp.clip(c - WIN_COLS // 2, 0, GRID_W - WIN_COLS)
    in_win = (c[None, :] >= c0[:, None]) & (c[None, :] < c0[:, None] + WIN_COLS)
    dr_idx = key_rows - r[:, None] + WIN_ROWS_MAX - 1
    dc_idx = jnp.clip(c[None, :] - c[:, None] + WIN_COLS - 1, 0, 2 * WIN_COLS - 2)
    bias = rpb[:, dr_idx[:, None, :, None], dc_idx[None, :, None, :]].astype(jnp.float32)
    qg = q.reshape(b, rows, GRID_W, h, hd)
    kb = k.reshape(b, rows, GRID_W, h, hd)[:, key_rows]
    vb = v.reshape(b, rows, GRID_W, h, hd)[:, key_rows]
    scores = jnp.einsum('brqhd,brwkhd->bhrqwk', qg, kb).astype(jnp.float32) * (hd ** -0.5) + bias[None]
    scores = jnp.where(in_win[None, None, None, :, None, :], scores, -jnp.inf)
    probs = jax.nn.softmax(scores.reshape(b, h, rows, GRID_W, win_r * GRID_W), axis=-1)
    probs = probs.reshape(b, h, rows, GRID_W, win_r, GRID_W).astype(v.dtype)
    out = jnp.einsum('bhrqwk,brwkhd->brqhd', probs, vb)
    return out.reshape(b, s, h * hd)


def sinusoidal_position_features(length):
    t = jnp.linspace(0.0, 1.0, length, dtype=jnp.float32)[:, None]
    bands = (FILTER_EMB - 1) // 2
    w = 2.0 * math.pi * jnp.arange(length, dtype=jnp.float32)[:, None] / length
    f = jnp.linspace(1e-4, bands - 1, bands, dtype=jnp.float32)[None, :]
    z = jnp.concatenate([t, jnp.cos(f * w), -jnp.sin(f * w)], axis=-1)
    return t, z


def implicit_filters(length, w1, b1, w2, b2, w3, freq):
    f32 = jnp.float32
    t, z = sinusoidal_position_features(length)
    fr = freq.astype(f32)
    hid = jnp.sin(fr * (z @ w1.astype(f32) + b1.astype(f32)))
    hid = jnp.sin(fr * (hid @ w2.astype(f32) + b2.astype(f32)))
    filt = (hid @ w3.astype(f32)).reshape(length, HYENA_ORDER, N_DIRS, HYENA_WIDTH)
    max_decay = math.log(DECAY_TARGET) / FAST_DECAY_PCT
    min_decay = math.log(DECAY_TARGET) / SLOW_DECAY_PCT
    deltas = jnp.abs(jnp.linspace(min_decay, max_decay, HYENA_WIDTH, dtype=f32))
    window = jnp.exp(-t * deltas[None, :])
    return filt * window[:, None, None, :]


def bidirectional_fft_conv(u, h_fwd, h_bwd, skip):
    length = u.shape[1]
    n = 2 * length
    taps = jnp.concatenate([h_fwd, jnp.zeros_like(h_fwd[:1]), h_bwd[:0:-1]], axis=0)
    uf = u.astype(jnp.float32)
    spec = jnp.fft.rfft(uf, n=n, axis=1) * jnp.fft.rfft(taps, n=n, axis=0)[None]
    y = jnp.fft.irfft(spec, n=n, axis=1)[:, :length]
    return (y + uf * skip.astype(jnp.float32)).astype(u.dtype)


def short_conv(u, w, bias):
    length = u.shape[1]
    half = SHORT_CONV_W // 2
    up = jnp.pad(u, ((0, 0), (half, SHORT_CONV_W - 1 - half), (0, 0)))
    out = bias
    for j in range(SHORT_CONV_W):
        out = out + up[:, j:j + length] * w[j]
    return out


def hyena_mixer(u, conv_w, conv_b, filters, filt_bias):
    u = short_conv(u, conv_w, conv_b)
    z = u[..., HYENA_ORDER * HYENA_WIDTH:]
    for o in range(HYENA_ORDER):
        gate = u[..., o * HYENA_WIDTH:(o + 1) * HYENA_WIDTH]
        z = gate * bidirectional_fft_conv(z, filters[:, o, 0], filters[:, o, 1], filt_bias[o])
    return z


def expert_choice_moe(h, w_router, w_gate, w_up, w_down):
    b, s, d = h.shape
    cap = EC_CAPACITY_FACTOR * s // N_EXPERTS
    logits = jnp.einsum('bsd,de->bse', h, w_router).astype(jnp.float32)
    affinity = jax.nn.softmax(logits, axis=-1)
    gate, idx = lax.top_k(jnp.swapaxes(affinity, 1, 2), cap)
    xg = jax.vmap(lambda hb, ib: hb[ib])(h, idx)
    a = jnp.einsum('becd,edf->becf', xg, w_gate)
    up = jnp.einsum('becd,edf->becf', xg, w_up)
    ye = jnp.einsum('becf,efd->becd', jax.nn.silu(a) * up, w_down)
    ye = ye * gate[..., None].astype(ye.dtype)
    return jax.vmap(lambda yb, ib: jnp.zeros((s, d), yb.dtype).at[ib.reshape(-1)].add(yb.reshape(-1, d)))(ye, idx)


def setup_inputs(seed: int = 0) -> dict:
    key = jax.random.key(seed)
    ks = jax.random.split(key, 24)
    f32 = jnp.float32

    def nrm(k, shape, scale):
        return jax.random.normal(k, shape, f32) * scale

    def gain(k, shape):
        return 1.0 + nrm(k, shape, 0.01)

    L = DEPTH
    return {
        'x': nrm(ks[0], (BATCH, SEQ, D_MODEL), 1.0),
        'mix_norm_g': gain(ks[1], (L, D_MODEL)),
        'w_in': nrm(ks[2], (L, D_MODEL, IN_WIDTH), D_MODEL ** -0.5),
        'q_norm_g': gain(ks[3], (L, HEAD_DIM)),
        'k_norm_g': gain(ks[4], (L, HEAD_DIM)),
        'rpb': nrm(ks[5], (L, N_ATTN_HEADS, 2 * WIN_ROWS_MAX - 1, 2 * WIN_COLS - 1), 0.02),
        'conv_w': nrm(ks[6], (L, SHORT_CONV_W, (HYENA_ORDER + 1) * HYENA_WIDTH), SHORT_CONV_W ** -0.5),
        'conv_b': nrm(ks[7], (L, (HYENA_ORDER + 1) * HYENA_WIDTH), 0.02),
        'filt_w1': nrm(ks[8], (L, FILTER_EMB, FILTER_HIDDEN), FILTER_EMB ** -0.5),
        'filt_b1': nrm(ks[9], (L, FILTER_HIDDEN), 0.02),
        'filt_w2': nrm(ks[10], (L, FILTER_HIDDEN, FILTER_HIDDEN), FILTER_HIDDEN ** -0.5),
        'filt_b2': nrm(ks[11], (L, FILTER_HIDDEN), 0.02),
        'filt_w3': nrm(ks[12], (L, FILTER_HIDDEN, HYENA_ORDER * N_DIRS * HYENA_WIDTH), FILTER_HIDDEN ** -0.5),
        'filt_freq': gain(ks[13], (L, FILTER_HIDDEN)),
        'filt_bias': nrm(ks[14], (L, HYENA_ORDER, HYENA_WIDTH), 0.5),
        'attn_out_g': gain(ks[15], (L, ATTN_WIDTH)),
        'hyena_out_g': gain(ks[16], (L, HYENA_WIDTH)),
        'w_out': nrm(ks[17], (L, D_MODEL, D_MODEL), D_MODEL ** -0.5),
        'ffn_norm_g': gain(ks[18], (L, D_MODEL)),
        'w_router': nrm(ks[19], (L, D_MODEL, N_EXPERTS), D_MODEL ** -0.5),
        'w_gate': nrm(ks[20], (L, N_EXPERTS, D_MODEL, EXPERT_FF), D_MODEL ** -0.5),
        'w_up': nrm(ks[21], (L, N_EXPERTS, D_MODEL, EXPERT_FF), D_MODEL ** -0.5),
        'w_down': nrm(ks[22], (L, N_EXPERTS, EXPERT_FF, D_MODEL), EXPERT_FF ** -0.5),
    }


def reference(x, mix_norm_g, w_in, q_norm_g, k_norm_g, rpb, conv_w, conv_b, filt_w1, filt_b1, filt_w2, filt_b2, filt_w3, filt_freq, filt_bias, attn_out_g, hyena_out_g, w_out, ffn_norm_g, w_router, w_gate, w_up, w_down):
    b, s, _ = x.shape
    for i in range(DEPTH):
        h = rms_norm(x, mix_norm_g[i])
        proj = jnp.einsum('bsd,de->bse', h, w_in[i])
        q, k, v = jnp.split(proj[..., :3 * ATTN_WIDTH], 3, axis=-1)
        q = rms_norm(q.reshape(b, s, N_ATTN_HEADS, HEAD_DIM), q_norm_g[i])
        k = rms_norm(k.reshape(b, s, N_ATTN_HEADS, HEAD_DIM), k_norm_g[i])
        v = v.reshape(b, s, N_ATTN_HEADS, HEAD_DIM)
        attn = neighbourhood_attention(q, k, v, rpb[i])
        filters = implicit_filters(s, filt_w1[i], filt_b1[i], filt_w2[i], filt_b2[i], filt_w3[i], filt_freq[i])
        hy = hyena_mixer(proj[..., 3 * ATTN_WIDTH:], conv_w[i], conv_b[i], filters, filt_bias[i])
        mixed = jnp.concatenate([group_rms_norm(attn, attn_out_g[i], N_ATTN_HEADS),
                                 group_rms_norm(hy, hyena_out_g[i], N_HYENA_GROUPS)], axis=-1)
        x = x + jnp.einsum('bsd,de->bse', mixed, w_out[i])
        x = x + expert_choice_moe(rms_norm(x, ffn_norm_g[i]), w_router[i], w_gate[i], w_up[i], w_down[i])
    return x
```

```python
import math
import numpy as np
import ml_dtypes
import numpy as np
import concourse.bass as bass
import concourse.mybir as mybir
from concourse.bass_utils import run_bass_kernel_spmd

F32 = mybir.dt.float32
BF16 = mybir.dt.bfloat16
I32 = mybir.dt.int32
AF = mybir.ActivationFunctionType
ALU = mybir.AluOpType
AX = mybir.AxisListType

ENGS = ("pe", "act", "dve", "pool", "sp")


class Buf:
    __slots__ = ("name", "lw", "rd")

    def __init__(self, name):
        self.name = name
        self.lw = None
        self.rd = {}


class Prog:
    SEM_ROLL = 30000
    N_DMA_SEMS = 40

    def __init__(self, nc, same_engine_sync=True):
        self.nc = nc
        self.same = same_engine_sync
        self.streams = {e: [] for e in ENGS}
        self.sems = {}
        self.cnt = {}
        self.eng_semkey = {}
        self.eng_epoch = {e: 0 for e in ENGS}
        self.waited = {e: {} for e in ENGS}
        self.dma_rr = 0
        self.dma_last = {}
        self._stack = []
        for e in ENGS:
            self._new_eng_sem(e)
        for i in range(self.N_DMA_SEMS):
            self._alloc(("dma", i))

    def _alloc(self, key):
        cm = self.nc.semaphore("s_" + "_".join(str(k) for k in key))
        h = cm.__enter__()
        self._stack.append(cm)
        self.sems[key] = h
        self.cnt[key] = 0
        return h

    def _new_eng_sem(self, e):
        key = ("eng", e, self.eng_epoch[e])
        self.eng_epoch[e] += 1
        self._alloc(key)
        self.eng_semkey[e] = key

    def buf(self, name):
        return Buf(name)

    def op(self, eng, fn, reads=(), writes=(), dma=False):
        waits = {}

        def need(tag):
            if tag is None:
                return
            k, v = tag
            if (not self.same) and k[0] == "eng" and k[1] == eng:
                return
            if eng == "pe" and k[0] == "eng" and k[1] == "pe":
                return
            if self.waited[eng].get(k, 0) >= v:
                return
            if waits.get(k, 0) < v:
                waits[k] = v

        for r in reads:
            need(r.lw)
        for w in writes:
            need(w.lw)
            for k, v in w.rd.items():
                need((k, v))
        if dma:
            key = ("dma", self.dma_rr)
            self.dma_rr = (self.dma_rr + 1) % self.N_DMA_SEMS
            need(self.dma_last.get(key))
            inc = 16
        else:
            key = self.eng_semkey[eng]
            if self.cnt[key] >= self.SEM_ROLL:
                self._new_eng_sem(eng)
                key = self.eng_semkey[eng]
            inc = 1
        for k, v in waits.items():
            self.waited[eng][k] = v
        self.cnt[key] += inc
        tag = (key, self.cnt[key])
        if dma:
            self.dma_last[key] = tag
        self.streams[eng].append((list(waits.items()), fn, key, inc))
        for r in reads:
            if r.rd.get(key, 0) < tag[1]:
                r.rd[key] = tag[1]
        for w in writes:
            w.lw = tag
            w.rd = {}
        return tag

    def finish(self, out_bufs):
        waits = {}
        for b in out_bufs:
            if b.lw is not None:
                k, v = b.lw
                waits[k] = max(waits.get(k, 0), v)
        self.streams["sp"].append((list(waits.items()), None, None, 0))

    def emit(self):
        nc = self.nc
        engmap = {"pe": "tensor", "act": "scalar", "dve": "vector", "pool": "gpsimd", "sp": "sync"}
        with nc.Block() as block:
            for e in ENGS:
                stream = self.streams[e]

                def body(eng, stream=stream):
                    for waits, fn, key, inc in stream:
                        for k, v in waits:
                            eng.wait_ge(self.sems[k], v)
                        if fn is not None:
                            ins = fn(eng)
                            ins.then_inc(self.sems[key], inc)

                getattr(block, engmap[e])(body)
        for cm in reversed(self._stack):
            cm.__exit__(None, None, None)
        self._stack = []


class T:
    __slots__ = ("t", "b")

    def __init__(self, t, b):
        self.t = t
        self.b = b


def _tile(self, name, shape, dtype, psum=False):
    cm = (self.nc.psum_tensor if psum else self.nc.sbuf_tensor)(name, list(shape), dtype)
    h = cm.__enter__()
    (self._pstack if psum else self._tstack).append(cm)
    return T(h, Buf(name))


def _free_psum(self):
    for cm in reversed(self._pstack):
        cm.__exit__(None, None, None)
    self._pstack = []


def _free_tiles(self):
    self.free_psum()
    for cm in reversed(self._tstack):
        cm.__exit__(None, None, None)
    self._tstack = []


def _barrier(self):
    tags = []
    for e in ENGS:
        k = self.eng_semkey[e]
        if self.cnt[k] > 0:
            tags.append((k, self.cnt[k]))
    for k, t in self.dma_last.items():
        tags.append(t)
    for e in ENGS:
        waits = []
        for k, v in tags:
            if k[0] == "eng" and k[1] == e:
                continue
            if self.waited[e].get(k, 0) >= v:
                continue
            self.waited[e][k] = v
            waits.append((k, v))
        if waits:
            self.streams[e].append((waits, None, None, 0))


Prog.tile = _tile
Prog.free_tiles = _free_tiles
Prog.free_psum = _free_psum
Prog.barrier = _barrier
Prog.SEM_ROLL = 10 ** 9
_old_init = Prog.__init__


def _init(self, nc, same_engine_sync=True):
    _old_init(self, nc, same_engine_sync)
    self._tstack = []
    self._pstack = []
    self.xTs_b = Buf('xTs')
    self.out_b = Buf('out')


Prog.__init__ = _init


def _mark(self):
    return len(self._tstack)


def _free_to(self, m):
    self.free_psum()
    while len(self._tstack) > m:
        self._tstack.pop().__exit__(None, None, None)


Prog.mark = _mark
Prog.free_to = _free_to

import math

EPS = 1e-6


def att_tables(rpb_h):
    BT = np.zeros((25, 128, 128), np.float32)
    MK = np.zeros((25, 128, 128), np.float32)
    reps = [0, 1, 2, 30, 31]
    kk = np.arange(128)
    a = kk // 64
    kc = kk % 64
    for cls, i in enumerate(reps):
        j0 = min(max(i - 2, 0), 27)
        for jj in range(5):
            j = j0 + jj
            kr = (2 * j + a)[:, None]
            qr = (2 * i + a)[None, :]
            qc = kc[None, :]
            kcc = kc[:, None]
            kr0 = np.clip(qr - 4, 0, 56)
            vr = (kr >= kr0) & (kr < kr0 + 8)
            c0 = np.clip(qc - 8, 0, 48)
            vc = (kcc >= c0) & (kcc < c0 + 16)
            valid = vr & vc
            dr = np.clip(kr - qr + 7, 0, 14)
            dc = np.clip(kcc - qc + 15, 0, 30)
            vals = rpb_h[dr, dc]
            BT[cls * 5 + jj] = np.where(valid, vals, 0.0)
            MK[cls * 5 + jj] = valid.astype(np.float32)
    BT = np.ascontiguousarray(BT.transpose(1, 0, 2).reshape(128, 25 * 128))
    MK = np.ascontiguousarray(MK.transpose(1, 0, 2).reshape(128, 25 * 128))
    return BT, MK


def cls_j0(i):
    j0 = min(max(i - 2, 0), 27)
    cls = {0: 0, 1: 1, 30: 3, 31: 4}.get(i, 2)
    return cls, j0


def phase_proj_attn(nc, P, x, watt, gmix, gqk, bt, mk, mixT, xTs, NBLK=16, why=None, rawD=None):
    NT = NBLK * 4
    identf = P.tile("identf", [128, 128], F32)
    ident = P.tile("ident", [128, 128], BF16)
    onesb = P.tile("onesb", [128, 128], BF16)
    wst = P.tile("wst", [128, 6144], F32)
    Wh = P.tile("Wh", [128, 6144], BF16)
    rawst = [P.tile("rawst%d" % i, [128, 512], F32) for i in range(2)]
    Wa = P.tile("Wa", [128, 6144], BF16)
    gm = P.tile("gm", [128, 16], F32)
    gq = P.tile("gq", [128, 2], F32)
    qT = P.tile("qT", [128, 8192], BF16)
    kT = P.tile("kT", [128, 8192], BF16)
    V1 = P.tile("V1", [128, 64 * 129], BF16)
    Et = P.tile("Et", [128, 3200], F32)
    mkt = P.tile("mkt", [128, 3200], F32)
    ssq = P.tile("ssq", [128, 256], F32)
    xts = [P.tile("xt%d" % i, [128, 2048], F32) for i in range(2)]
    xn = P.tile("xn", [128, 2048], BF16)
    xTb = [P.tile("xT%d" % i, [128, 16 * 512], BF16) for i in range(2)]
    sq = P.tile("sq", [128, 512], BF16)
    rr = P.tile("rr", [128, 512], F32)
    pTs = [P.tile("pT%d" % i, [128, 2048], BF16, psum=True) for i in range(2)]
    pqk = [P.tile("pqk%d" % i, [128, 512], F32, psum=True) for i in range(2)]
    pss = P.tile("pss", [128, 512], F32, psum=True)
    pv = P.tile("pv", [128, 512], F32, psum=True)

    P.op("pool", lambda e: e.memset(identf.t[:], 1.0), writes=[identf.b])
    P.op("pool", lambda e: e.affine_select(out=identf.t[:], in_=identf.t[:], pattern=[[-1, 128]], compare_op=ALU.is_equal, fill=0.0, base=0, channel_multiplier=1), reads=[identf.b], writes=[identf.b])
    P.op("dve", lambda e: e.tensor_copy(out=ident.t[:], in_=identf.t[:]), reads=[identf.b], writes=[ident.b])
    P.op("pool", lambda e: e.memset(identf.t[:], 1.0), reads=[identf.b], writes=[identf.b])
    P.op("dve", lambda e: e.tensor_copy(out=onesb.t[:], in_=identf.t[:]), reads=[identf.b], writes=[onesb.b])
    P.op("dve", lambda e: e.memset(ssq.t[:], 0.0), writes=[ssq.b])
    V1v = V1.t[:].rearrange("p (t c) -> p t c", c=129)
    P.op("dve", lambda e: e.tensor_copy(out=V1v[:, :, 128:129], in_=identf.t[:, 0:64].unsqueeze(2)), reads=[identf.b], writes=[V1.b])
    P.op("sp", lambda e: e.dma_start(out=gm.t[:], in_=gmix[:, :]), writes=[gm.b], dma=True)
    P.op("sp", lambda e: e.dma_start(out=gq.t[:], in_=gqk[:, :]), writes=[gq.b], dma=True)
    P.op("sp", lambda e: e.dma_start(out=wst.t[:, 0:6144].rearrange("p (k c) -> p k c", k=16), in_=watt.rearrange("(k p) c -> p k c", p=128)), writes=[wst.b], dma=True)
    for dk in range(16):
        P.op("act", lambda e, dk=dk: e.activation(out=Wa.t[:, dk * 384:(dk + 1) * 384], in_=wst.t[:, dk * 384:(dk + 1) * 384], func=AF.Copy, scale=gm.t[:, dk:dk + 1]), reads=[wst.b, gm.b], writes=[Wa.b])
    if why is not None:
        P.op("sp", lambda e: e.dma_start(out=wst.t[:, 0:6144].rearrange("p (k c) -> p k c", k=16), in_=why.rearrange("(k p) c -> p k c", p=128)), writes=[wst.b], dma=True)
        for dk in range(16):
            P.op("act", lambda e, dk=dk: e.activation(out=Wh.t[:, dk * 384:(dk + 1) * 384], in_=wst.t[:, dk * 384:(dk + 1) * 384], func=AF.Copy, scale=gm.t[:, dk:dk + 1]), reads=[wst.b, gm.b], writes=[Wh.b])
    P.op("sp", lambda e: e.dma_start(out=wst.t[:, 0:3200], in_=bt[:, :]), writes=[wst.b], dma=True)
    P.op("sp", lambda e: e.dma_start(out=mkt.t[:], in_=mk[:, :]), writes=[mkt.b], dma=True)
    P.op("act", lambda e: e.activation(out=Et.t[:], in_=wst.t[:, 0:3200], func=AF.Exp), reads=[wst.b], writes=[Et.b])
    P.op("dve", lambda e: e.tensor_tensor(out=Et.t[:], in0=Et.t[:], in1=mkt.t[:], op=ALU.mult), reads=[Et.b, mkt.b], writes=[Et.b])

    Wav = Wa.t[:].rearrange("p (k c) -> p k c", k=16)
    for blk in range(NBLK):
        xT = xTb[blk % 2]
        xTv = xT.t[:].rearrange("p (k t) -> p k t", k=16)
        for tl in range(4):
            ti = blk * 4 + tl
            tok0 = ti * 128
            xt = xts[ti % 2]
            pT = pTs[ti % 2]
            P.op("sp", lambda e, xt=xt, tok0=tok0: e.dma_start(out=xt.t[:], in_=x[tok0:tok0 + 128, :]), writes=[xt.b], dma=True)
            c = ti
            P.op("act", lambda e, xt=xt, c=c: e.activation(out=xn.t[:], in_=xt.t[:], func=AF.Square, accum_out=ssq.t[:, c:c + 1]), reads=[xt.b], writes=[xn.b, ssq.b])
            P.op("act", lambda e, c=c: e.activation(out=ssq.t[:, c:c + 1], in_=ssq.t[:, c:c + 1], func=AF.Sqrt, scale=1.0 / 2048, bias=EPS), reads=[ssq.b], writes=[ssq.b])
            P.op("dve", lambda e, c=c: e.reciprocal(out=ssq.t[:, c:c + 1], in_=ssq.t[:, c:c + 1]), reads=[ssq.b], writes=[ssq.b])
            P.op("dve", lambda e, xt=xt, c=c: e.tensor_scalar(out=xn.t[:], in0=xt.t[:], scalar1=ssq.t[:, c:c + 1], scalar2=None, op0=ALU.mult), reads=[xt.b, ssq.b], writes=[xn.b])
            for dk in range(16):
                P.op("pe", lambda e, pT=pT, dk=dk: e.transpose(out=pT.t[:, dk * 128:(dk + 1) * 128], in_=xn.t[:, dk * 128:(dk + 1) * 128], identity=ident.t[:]), reads=[xn.b, ident.b], writes=[pT.b])
            ev = "act" if tl % 2 == 0 else "dve"
            if ev == "act":
                P.op("act", lambda e, pT=pT, xTv=xTv, tl=tl: e.activation(out=xTv[:, :, tl * 128:(tl + 1) * 128], in_=pT.t[:].rearrange("p (k t) -> p k t", k=16), func=AF.Copy), reads=[pT.b], writes=[xT.b])
            else:
                P.op("dve", lambda e, pT=pT, xTv=xTv, tl=tl: e.tensor_copy(out=xTv[:, :, tl * 128:(tl + 1) * 128], in_=pT.t[:].rearrange("p (k t) -> p k t", k=16)), reads=[pT.b], writes=[xT.b])
        if xTs is not None:
            P.op("sp", lambda e, xT=xT, blk=blk: e.dma_start(out=xTs[blk, :, :], in_=xT.t[:]), reads=[xT.b], writes=[P.xTs_b], dma=True)
        for which, dst in ((0, qT), (1, kT)):
            pq = pqk[which]
            for dk in range(16):
                P.op("pe", lambda e, pq=pq, dk=dk, which=which, xTv=xTv: e.matmul(pq.t[:], lhsT=Wav[:, dk, which * 128:(which + 1) * 128], rhs=xTv[:, dk, :], start=(dk == 0), stop=(dk == 15)), reads=[Wa.b, xT.b], writes=[pq.b])
            P.op("act", lambda e, pq=pq: e.activation(out=sq.t[:], in_=pq.t[:], func=AF.Square), reads=[pq.b], writes=[sq.b])
            P.op("pe", lambda e: e.matmul(pss.t[:], lhsT=onesb.t[:], rhs=sq.t[:], start=True, stop=True), reads=[onesb.b, sq.b], writes=[pss.b])
            P.op("act", lambda e: e.activation(out=rr.t[:], in_=pss.t[:], func=AF.Sqrt, scale=1.0 / 128, bias=EPS), reads=[pss.b], writes=[rr.b])
            P.op("dve", lambda e: e.reciprocal(out=rr.t[:], in_=rr.t[:]), reads=[rr.b], writes=[rr.b])
            P.op("dve", lambda e, pq=pq, dst=dst, which=which, blk=blk: e.scalar_tensor_tensor(out=dst.t[:, blk * 512:(blk + 1) * 512], in0=pq.t[:], scalar=gq.t[:, which:which + 1], in1=rr.t[:], op0=ALU.mult, op1=ALU.mult), reads=[pq.b, gq.b, rr.b], writes=[dst.b])
        if why is not None:
            Whv = Wh.t[:].rearrange("p (k c) -> p k c", k=16)
            for s3 in range(3):
                pq = pqk[s3 % 2]
                rs_ = rawst[s3 % 2]
                for dk in range(16):
                    P.op("pe", lambda e, pq=pq, dk=dk, s3=s3, xTv=xTv, Whv=Whv: e.matmul(pq.t[:], lhsT=Whv[:, dk, s3 * 128:(s3 + 1) * 128], rhs=xTv[:, dk, :], start=(dk == 0), stop=(dk == 15)), reads=[Wh.b, xT.b], writes=[pq.b])
                P.op("act", lambda e, pq=pq, rs_=rs_: e.activation(out=rs_.t[:], in_=pq.t[:], func=AF.Copy), reads=[pq.b], writes=[rs_.b])
                P.op("sp", lambda e, rs_=rs_, s3=s3, blk=blk: e.dma_start(out=rawD[s3, :, blk * 512:(blk + 1) * 512], in_=rs_.t[:]), reads=[rs_.b], writes=[P.xTs_b], dma=True)
        for tl in range(4):
            ti = blk * 4 + tl
            for dk in range(16):
                P.op("pe", lambda e, dk=dk, tl=tl, xTv=xTv: e.matmul(pv.t[:, 0:128], lhsT=xTv[:, dk, tl * 128:(tl + 1) * 128], rhs=Wav[:, dk, 256:384], start=(dk == 0), stop=(dk == 15)), reads=[Wa.b, xT.b], writes=[pv.b])
            P.op("act", lambda e, ti=ti: e.activation(out=V1.t[:, ti * 129:ti * 129 + 128], in_=pv.t[:, 0:128], func=AF.Copy), reads=[pv.b], writes=[V1.b])

    P.barrier()
    P.free_psum()
    pS = [P.tile("pS%d" % i, [128, 1024], F32, psum=True) for i in range(2)]
    pO = [P.tile("pO%d" % i, [128, 512], F32, psum=True) for i in range(2)]
    pTt = P.tile("pTt", [128, 1024], BF16, psum=True)
    pexp = P.tile("pexp", [128, 640], F32)
    PT = [P.tile("PT%d" % i, [128, 640], BF16) for i in range(2)]
    of = P.tile("of", [128, 128], F32)
    onb = P.tile("onb", [128, 128], BF16)
    mixA = P.tile("mixA", [128, 8192], BF16)
    scale = 128 ** -0.5
    nb = NT // 32
    qi = 0
    for b in range(nb):
        for i in range(32):
            cls, j0 = cls_j0(i)
            qtok = b * 4096 + i * 128
            ps = pS[qi % 2]
            po = pO[qi % 2]
            pt = PT[qi % 2]
            for jj in range(5):
                ktok = b * 4096 + (j0 + jj) * 128
                P.op("pe", lambda e, ps=ps, jj=jj, ktok=ktok, qtok=qtok: e.matmul(ps.t[:, jj * 128:(jj + 1) * 128], lhsT=kT.t[:, ktok:ktok + 128], rhs=qT.t[:, qtok:qtok + 128], start=True, stop=True), reads=[kT.b, qT.b], writes=[ps.b])
            P.op("act", lambda e, ps=ps: e.activation(out=pexp.t[:], in_=ps.t[:, 0:640], func=AF.Exp, scale=scale), reads=[ps.b], writes=[pexp.b])
            P.op("dve", lambda e, pt=pt, cls=cls: e.tensor_tensor(out=pt.t[:], in0=pexp.t[:], in1=Et.t[:, cls * 640:(cls + 1) * 640], op=ALU.mult), reads=[pexp.b, Et.b], writes=[pt.b])
            for jj in range(5):
                vt = b * 32 + j0 + jj
                P.op("pe", lambda e, po=po, pt=pt, jj=jj, vt=vt: e.matmul(po.t[:, 0:129], lhsT=pt.t[:, jj * 128:(jj + 1) * 128], rhs=V1.t[:, vt * 129:(vt + 1) * 129], start=(jj == 0), stop=(jj == 4)), reads=[pt.b, V1.b], writes=[po.b])
            c1 = 64 + qi
            c2 = 128 + qi
            P.op("dve", lambda e, po=po, c1=c1: e.reciprocal(out=ssq.t[:, c1:c1 + 1], in_=po.t[:, 128:129]), reads=[po.b], writes=[ssq.b])
            P.op("dve", lambda e, po=po, c1=c1: e.tensor_scalar(out=of.t[:], in0=po.t[:, 0:128], scalar1=ssq.t[:, c1:c1 + 1], scalar2=None, op0=ALU.mult), reads=[po.b, ssq.b], writes=[of.b])
            P.op("act", lambda e, c2=c2: e.activation(out=onb.t[:], in_=of.t[:], func=AF.Square, accum_out=ssq.t[:, c2:c2 + 1]), reads=[of.b], writes=[onb.b, ssq.b])
            P.op("act", lambda e, c2=c2: e.activation(out=ssq.t[:, c2:c2 + 1], in_=ssq.t[:, c2:c2 + 1], func=AF.Sqrt, scale=1.0 / 128, bias=EPS), reads=[ssq.b], writes=[ssq.b])
            P.op("dve", lambda e, c2=c2: e.reciprocal(out=ssq.t[:, c2:c2 + 1], in_=ssq.t[:, c2:c2 + 1]), reads=[ssq.b], writes=[ssq.b])
            P.op("dve", lambda e, c2=c2: e.tensor_scalar(out=onb.t[:], in0=of.t[:], scalar1=ssq.t[:, c2:c2 + 1], scalar2=None, op0=ALU.mult), reads=[of.b, ssq.b], writes=[onb.b])
            P.op("pe", lambda e: e.transpose(out=pTt.t[:, 0:128], in_=onb.t[:], identity=ident.t[:]), reads=[onb.b, ident.b], writes=[pTt.b])
            P.op("act", lambda e, qtok=qtok: e.activation(out=mixA.t[:, qtok:qtok + 128], in_=pTt.t[:, 0:128], func=AF.Copy), reads=[pTt.b], writes=[mixA.b])
            qi += 1
    ntok = NT * 128
    P.op("sp", lambda e: e.dma_start(out=mixT[0:128, 0:ntok], in_=mixA.t[:, 0:ntok]), reads=[mixA.b], writes=[P.out_b], dma=True)

import ml_dtypes
BF = ml_dtypes.bfloat16
N_FFT = 8192; L_SEQ = 4096

def fft_consts():
    a = np.arange(128)[:, None].astype(np.float64); f1 = np.arange(64)[None, :].astype(np.float64)
    phi = 2 * np.pi * a * (f1 + 0.5) / 128
    F3 = np.concatenate([np.cos(phi), -np.sin(phi)], axis=1)
    F128 = F3.astype(np.float32)
    Fblk = np.zeros((128, 256), np.float64)
    Fblk[0:64, 0:128] = F3[0:64]
    Fblk[64:128, 128:256] = F3[0:64]
    p = np.arange(64)[:, None].astype(np.float64); f2 = np.arange(64)[None, :].astype(np.float64)
    Gf = np.zeros((128, 64, 3, 128), np.float64)
    Gi = np.zeros((128, 64, 3, 128), np.float64)
    for f1i in range(64):
        ang = -2 * np.pi * p * (f1i + 128 * f2 + 0.5) / N_FFT
        Gre = np.cos(ang); Gim = np.sin(ang)
        for cl in range(2):
            s = slice(cl * 64, cl * 64 + 64)
            Gf[s, f1i, 0, s] = Gre; Gf[s, f1i, 1, s] = Gim; Gf[s, f1i, 2, s] = -Gim
            Gi[s, f1i, 0, s] = Gre.T; Gi[s, f1i, 1, s] = Gim.T; Gi[s, f1i, 2, s] = -Gim.T
    aa = np.arange(64)[None, :].astype(np.float64); ff = np.arange(64)[:, None].astype(np.float64)
    th = 2 * np.pi * aa * (ff + 0.5) / 128
    Einv = np.zeros((64, 2, 64), np.float64)
    Einv[:, 0, :] = (2.0 / N_FFT) * np.cos(th); Einv[:, 1, :] = -(2.0 / N_FFT) * np.sin(th)
    return dict(F128=F128.astype(BF), Fblk=Fblk.astype(BF), Gf=Gf.reshape(128, -1).astype(BF), Gi=Gi.reshape(128, -1).astype(BF), Einv=Einv.reshape(128, 64).astype(BF))

def pos_consts(h):
    t = np.linspace(0.0, 1.0, L_SEQ, dtype=np.float32)[:, None]
    bands = 16
    w = (2.0 * math.pi * np.arange(L_SEQ, dtype=np.float32)[:, None] / L_SEQ).astype(np.float32)
    f = np.linspace(1e-4, bands - 1, bands, dtype=np.float32)[None, :]
    z = np.concatenate([t, np.cos(f * w), -np.sin(f * w)], axis=-1).astype(np.float32)
    j = np.arange(N_FFT)
    pos = np.where(j < L_SEQ, j, np.where(j == L_SEQ, 0, N_FFT - j))
    zposT = np.ascontiguousarray(z[pos].T)
    max_decay = math.log(1e-2) / 0.3; min_decay = math.log(1e-2) / 1.5
    deltas = np.abs(np.linspace(min_decay, max_decay, 1024, dtype=np.float32))[128 * h:128 * h + 128]
    window = np.exp(-t * deltas[None, :]).astype(np.float32)
    sg = np.where(j < L_SEQ, 1.0, np.where(j == L_SEQ, 0.0, -1.0)).astype(np.float32)
    winsg = (window[pos] * sg[:, None]).reshape(128, 64, 128)
    return zposT, np.ascontiguousarray(winsg.reshape(128, 64 * 128))

import math
EPS = 1e-6
MAGIC = 12582912.0
TWO_PI = 2.0 * math.pi


def evac(P, i, out, in_, reads, writes):
    if i % 2 == 0:
        P.op("act", lambda e: e.activation(out=out, in_=in_, func=AF.Copy), reads=reads, writes=writes)
    else:
        P.op("dve", lambda e: e.tensor_copy(out=out, in_=in_), reads=reads, writes=writes)


def phase_hyena(nc, P, D, mixT, dbg=None, stage='all'):
    ident = P.tile("identh", [128, 128], BF16)
    identf = P.tile("identfh", [128, 128], F32)
    P.op("pool", lambda e: e.memset(identf.t[:], 1.0), writes=[identf.b])
    P.op("pool", lambda e: e.affine_select(out=identf.t[:], in_=identf.t[:], pattern=[[-1, 128]], compare_op=ALU.is_equal, fill=0.0, base=0, channel_multiplier=1), reads=[identf.b], writes=[identf.b])
    P.op("dve", lambda e: e.tensor_copy(out=ident.t[:], in_=identf.t[:]), reads=[identf.b], writes=[ident.b])
    zb = P.tile("zb", [128, 8192], BF16)
    g0b = P.tile("g0b", [128, 8192], BF16)
    g1b = P.tile("g1b", [128, 8192], BF16)
    cwt = P.tile("cwt", [128, 12], F32)
    bufB = P.tile("bufB", [128, 16384], BF16)
    mk1 = P.mark()
    P.op("sp", lambda e: e.dma_start(out=cwt.t[:], in_=D["cwb"][:, :]), writes=[cwt.b], dma=True)
    raw = P.tile("raw", [128, 8192], F32)
    cv = P.tile("cv", [128, 8192], F32)
    cvb = P.tile("cvb", [128, 8192], BF16)
    pTz = [P.tile("pTz%d" % i, [128, 1024], BF16, psum=True) for i in range(2)]
    dsts = [g0b, g1b, zb]
    n = 0
    for s in range(3):
        P.op("sp", lambda e, s=s: e.dma_start(out=raw.t[:], in_=D["rawD"][s, :, :]), writes=[raw.b], dma=True)
        P.op("act", lambda e, s=s: e.activation(out=cv.t[:], in_=raw.t[:], func=AF.Identity, scale=cwt.t[:, 3 * s + 1:3 * s + 2], bias=cwt.t[:, 9 + s:10 + s]), reads=[raw.b, cwt.b], writes=[cv.b])
        for b in range(2):
            o = b * 4096
            P.op("dve", lambda e, s=s, o=o: e.scalar_tensor_tensor(out=cv.t[:, o + 1:o + 4096], in0=raw.t[:, o:o + 4095], scalar=cwt.t[:, 3 * s:3 * s + 1], in1=cv.t[:, o + 1:o + 4096], op0=ALU.mult, op1=ALU.add), reads=[raw.b, cv.b, cwt.b], writes=[cv.b])
            P.op("dve", lambda e, s=s, o=o: e.scalar_tensor_tensor(out=cvb.t[:, o:o + 4095], in0=raw.t[:, o + 1:o + 4096], scalar=cwt.t[:, 3 * s + 2:3 * s + 3], in1=cv.t[:, o:o + 4095], op0=ALU.mult, op1=ALU.add), reads=[raw.b, cv.b, cwt.b], writes=[cvb.b])
            P.op("dve", lambda e, o=o: e.tensor_copy(out=cvb.t[:, o + 4095:o + 4096], in_=cv.t[:, o + 4095:o + 4096]), reads=[cv.b], writes=[cvb.b])
        cvv = cvb.t[:].rearrange("c (m p) -> c m p", p=64)
        dv = dsts[s].t[:].rearrange("q (c p) -> q c p", p=64)
        for p0 in range(0, 64, 8):
            pt = pTz[n % 2]
            for pp in range(8):
                P.op("pe", lambda e, pt=pt, pp=pp, p0=p0, cvv=cvv: e.transpose(out=pt.t[:, pp * 128:(pp + 1) * 128], in_=cvv[:, :, p0 + pp], identity=ident.t[:]), reads=[cvb.b, ident.b], writes=[pt.b])
            evac(P, n, dv[:, :, p0:p0 + 8], pt.t[:].rearrange("q (p c) -> q c p", p=8), [pt.b], [dsts[s].b])
            n += 1
    if dbg is not None:
        P.op("sp", lambda e: e.dma_start(out=dbg["zb"][:, :], in_=zb.t[:]), reads=[zb.b], writes=[P.out_b], dma=True)
    P.barrier()
    P.free_to(mk1)
    if stage == "h1":
        return
    h2 = P.tile("h2", [64, 8192], F32)
    w1t = P.tile("w1t", [33, 64], F32)
    w2t = P.tile("w2t", [64, 64], F32)
    w3t = P.tile("w3t", [64, 512], F32)
    fvt = P.tile("fvt", [64, 4], F32)
    zpc = [P.tile("zpc%d" % i, [33, 512], F32) for i in range(2)]
    h1c = P.tile("h1c", [64, 512], F32)
    uu = P.tile("uu", [64, 512], F32)
    kk = P.tile("kk", [64, 512], F32)
    winc = [P.tile("winc%d" % i, [128, 256], F32) for i in range(2)]
    tapsb = bufB
    pm = [P.tile("pm%d" % i, [128, 512], F32, psum=True) for i in range(2)]
    for t_, k_ in ((w1t, "w1"), (w2t, "w2"), (w3t, "w3h")):
        P.op("sp", lambda e, t_=t_, k_=k_: e.dma_start(out=t_.t[:], in_=D[k_][:, :]), writes=[t_.b], dma=True)
    P.op("sp", lambda e: e.dma_start(out=fvt.t[:, 0:3], in_=D["fv"][:, :]), writes=[fvt.b], dma=True)
    P.op("dve", lambda e: e.tensor_scalar(out=fvt.t[:, 1:3], in0=fvt.t[:, 1:3], scalar1=fvt.t[:, 0:1], scalar2=None, op0=ALU.mult), reads=[fvt.b], writes=[fvt.b])

    def sin_layer(src_ps, l, dst):
        P.op("dve", lambda e: e.tensor_scalar(out=uu.t[:], in0=src_ps.t[0:64, :], scalar1=fvt.t[:, 0:1], scalar2=fvt.t[:, l:l + 1], op0=ALU.mult, op1=ALU.add), reads=[src_ps.b, fvt.b], writes=[uu.b])
        P.op("dve", lambda e: e.tensor_scalar(out=kk.t[:], in0=uu.t[:], scalar1=1.0 / TWO_PI, scalar2=MAGIC, op0=ALU.mult, op1=ALU.add), reads=[uu.b], writes=[kk.b])
        P.op("dve", lambda e: e.tensor_scalar(out=kk.t[:], in0=kk.t[:], scalar1=-MAGIC, scalar2=-TWO_PI, op0=ALU.add, op1=ALU.mult), reads=[kk.b], writes=[kk.b])
        P.op("dve", lambda e: e.tensor_tensor(out=uu.t[:], in0=uu.t[:], in1=kk.t[:], op=ALU.add), reads=[uu.b, kk.b], writes=[uu.b])
        P.op("act", lambda e: e.activation(out=dst, in_=uu.t[:], func=AF.Sin), reads=[uu.b], writes=[h1c.b, h2.b])

    for ch in range(16):
        zp = zpc[ch % 2]
        P.op("sp", lambda e, zp=zp, ch=ch: e.dma_start(out=zp.t[:], in_=D["zposT"][:, ch * 512:(ch + 1) * 512]), writes=[zp.b], dma=True)
        P.op("pe", lambda e, zp=zp: e.matmul(pm[0].t[0:64, :], lhsT=w1t.t[:], rhs=zp.t[:], start=True, stop=True), reads=[w1t.b, zp.b], writes=[pm[0].b])
        sin_layer(pm[0], 1, h1c.t[:])
        P.op("pe", lambda e: e.matmul(pm[1].t[0:64, :], lhsT=w2t.t[:], rhs=h1c.t[:], start=True, stop=True), reads=[w2t.b, h1c.b], writes=[pm[1].b])
        sin_layer(pm[1], 2, h2.t[0:64, ch * 512:(ch + 1) * 512])
    h2v = h2.t[0:64, :].rearrange("k (a p) -> k a p", p=64)
    w3v = w3t.t[:].rearrange("k (o d c) -> k o d c", o=2, d=2)
    tapsv = tapsb.t[:].rearrange("a (o c p) -> a o c p", o=2, p=64)
    for p2 in range(32):
        pmt = pm[p2 % 2]
        wc = winc[p2 % 2]
        P.op("sp", lambda e, wc=wc, p2=p2: e.dma_start(out=wc.t[:], in_=D["winsg"][:, p2 * 256:(p2 + 1) * 256]), writes=[wc.b], dma=True)
        for pp in range(2):
            p = 2 * p2 + pp
            P.op("pe", lambda e, pmt=pmt, pp=pp, p=p: e.matmul(pmt.t[0:64, pp * 256:(pp + 1) * 256], lhsT=h2v[:, 0:64, p], rhs=w3v[:, :, 0, :], start=True, stop=True), reads=[h2.b, w3t.b], writes=[pmt.b])
            P.op("pe", lambda e, pmt=pmt, pp=pp, p=p: e.matmul(pmt.t[64:128, pp * 256:(pp + 1) * 256], lhsT=h2v[:, 64:128, p], rhs=w3v[:, :, 1, :], start=True, stop=True), reads=[h2.b, w3t.b], writes=[pmt.b])
        P.op("dve", lambda e, pmt=pmt, wc=wc, p2=p2: e.tensor_tensor(
            out=tapsv[:, :, :, 2 * p2:2 * p2 + 2].rearrange("a o c p -> a p o c"),
            in0=pmt.t[:].rearrange("a (p o c) -> a p o c", p=2, o=2),
            in1=wc.t[:].rearrange("a (p c) -> a p c", p=2).unsqueeze(2).to_broadcast([128, 2, 2, 128]), op=ALU.mult), reads=[pmt.b, wc.b], writes=[tapsb.b])
    if dbg is not None:
        P.op("sp", lambda e: e.dma_start(out=dbg["taps"][:, :], in_=tapsb.t[:]), reads=[tapsb.b], writes=[P.out_b], dma=True)
    P.barrier()
    P.free_to(mk1)
    if stage == "taps":
        return
    bufA = P.tile("bufA", [128, 16384], BF16)
    Hs = P.tile("Hs", [128, 16384], BF16)
    Gc = [P.tile("Gc%d" % i, [128, 8 * 384], BF16) for i in range(2)]
    Fblk = P.tile("Fblk_s", [128, 256], BF16)
    F128 = P.tile("F128_s", [128, 128], BF16)
    Einv = P.tile("Einv_s", [128, 64], BF16)
    skipb = P.tile("skipb_s", [128, 256], F32)
    m1 = P.tile("m1", [128, 1024], F32)
    m2 = P.tile("m2", [128, 1024], F32)
    n1 = P.tile("n1", [128, 1024], F32)
    n2 = P.tile("n2", [128, 1024], F32)
    st = P.tile("sth", [128, 64], F32)
    for t_, k_ in ((Fblk, "Fblk"), (F128, "F128"), (Einv, "Einv"), (skipb, "skipb")):
        P.op("sp", lambda e, t_=t_, k_=k_: e.dma_start(out=t_.t[:], in_=D[k_][:, :]), writes=[t_.b], dma=True)
    pA = [P.tile("pA%d" % i, [128, 512], F32, psum=True) for i in range(4)]
    pW = [P.tile("pW%d" % i, [128, 1024], BF16, psum=True) for i in range(2)]
    cnt = [0]

    def stageA(src, filt):
        for ch in range(64):
            pa = pA[cnt[0] % 4]
            if filt:
                for o in range(2):
                    P.op("pe", lambda e, pa=pa, o=o, ch=ch: e.matmul(pa.t[:, o * 128:(o + 1) * 128], lhsT=src.t[:, o * 8192 + ch * 128:o * 8192 + (ch + 1) * 128], rhs=F128.t[:], start=True, stop=True), reads=[src.b, F128.b], writes=[pa.b])
            else:
                P.op("pe", lambda e, pa=pa, ch=ch: e.matmul(pa.t[:, 0:256], lhsT=src.t[:, ch * 128:(ch + 1) * 128], rhs=Fblk.t[:], start=True, stop=True), reads=[src.b, Fblk.b], writes=[pa.b])
            evac(P, cnt[0], bufA.t[:, ch * 256:(ch + 1) * 256], pa.t[:, 0:256], [pa.b], [bufA.b])
            cnt[0] += 1

    def load_G(key, g8):
        gc = Gc[cnt[0] % 2]
        P.op("sp", lambda e, gc=gc: e.dma_start(out=gc.t[:], in_=D[key][:, g8 * 8 * 384:(g8 + 1) * 8 * 384]), writes=[gc.b], dma=True)
        return gc

    def stageC(dst):
        Yv = bufA.t[:].rearrange("q (c b r f) -> q c b r f", b=2, r=2, f=64)
        for g8 in range(8):
            gc = load_G("Gf", g8)
            gv = gc.t[:].rearrange("q (f m k) -> q f m k", m=3, k=128)
            for fi in range(8):
                f1 = g8 * 8 + fi
                pa = pA[cnt[0] % 4]
                half = (f1 % 2) * 256
                yre = Yv[:, :, :, 0, f1]
                yim = Yv[:, :, :, 1, f1]
                P.op("pe", lambda e, pa=pa, gv=gv, fi=fi, half=half, yre=yre: e.matmul(pa.t[:, half:half + 128], lhsT=gv[:, fi, 0, :], rhs=yre, start=True, stop=False), reads=[gc.b, bufA.b], writes=[pa.b])
                P.op("pe", lambda e, pa=pa, gv=gv, fi=fi, half=half, yim=yim: e.matmul(pa.t[:, half:half + 128], lhsT=gv[:, fi, 2, :], rhs=yim, start=False, stop=True), reads=[gc.b, bufA.b], writes=[pa.b])
                P.op("pe", lambda e, pa=pa, gv=gv, fi=fi, half=half, yre=yre: e.matmul(pa.t[:, half + 128:half + 256], lhsT=gv[:, fi, 1, :], rhs=yre, start=True, stop=False), reads=[gc.b, bufA.b], writes=[pa.b])
                P.op("pe", lambda e, pa=pa, gv=gv, fi=fi, half=half, yim=yim: e.matmul(pa.t[:, half + 128:half + 256], lhsT=gv[:, fi, 0, :], rhs=yim, start=False, stop=True), reads=[gc.b, bufA.b], writes=[pa.b])
                if f1 % 2 == 1:
                    evac(P, cnt[0], dst.t[:, (f1 - 1) * 256:(f1 + 1) * 256], pa.t[:], [pa.b], [dst.b])
                    cnt[0] += 1

    def stageCinv(src, dst):
        for g8 in range(8):
            gc = load_G("Gi", g8)
            gv = gc.t[:].rearrange("q (f m k) -> q f m k", m=3, k=128)
            for fi in range(8):
                f1 = g8 * 8 + fi
                pa = pA[cnt[0] % 4]
                half = (f1 % 2) * 256
                zre = src.t[:, f1 * 256:f1 * 256 + 128]
                zim = src.t[:, f1 * 256 + 128:f1 * 256 + 256]
                P.op("pe", lambda e, pa=pa, gv=gv, fi=fi, half=half, zre=zre: e.matmul(pa.t[:, half:half + 128], lhsT=gv[:, fi, 0, :], rhs=zre, start=True, stop=False), reads=[gc.b, src.b], writes=[pa.b])
                P.op("pe", lambda e, pa=pa, gv=gv, fi=fi, half=half, zim=zim: e.matmul(pa.t[:, half:half + 128], lhsT=gv[:, fi, 1, :], rhs=zim, start=False, stop=True), reads=[gc.b, src.b], writes=[pa.b])
                P.op("pe", lambda e, pa=pa, gv=gv, fi=fi, half=half, zim=zim: e.matmul(pa.t[:, half + 128:half + 256], lhsT=gv[:, fi, 0, :], rhs=zim, start=True, stop=False), reads=[gc.b, src.b], writes=[pa.b])
                P.op("pe", lambda e, pa=pa, gv=gv, fi=fi, half=half, zre=zre: e.matmul(pa.t[:, half + 128:half + 256], lhsT=gv[:, fi, 2, :], rhs=zre, start=False, stop=True), reads=[gc.b, src.b], writes=[pa.b])
                if f1 % 2 == 1:
                    evac(P, cnt[0], dst.t[:, (f1 - 1) * 256:(f1 + 1) * 256], pa.t[:], [pa.b], [dst.b])
                    cnt[0] += 1

    def specmul(X, o, Z):
        Xv = X.t[:].rearrange("q (f r c b) -> q f r c b", r=2, c=64, b=2)
        Hv = Hs.t[:].rearrange("q (f r c b) -> q f r c b", r=2, c=64, b=2)
        Zv = Z.t[:].rearrange("q (f r c b) -> q f r c b", r=2, c=64, b=2)
        def tv(t_):
            return t_.t[:].rearrange("q (f c b) -> q f c b", f=8, b=2)
        for f0 in range(0, 64, 8):
            x0 = Xv[:, f0:f0 + 8, 0, :, :]
            x1 = Xv[:, f0:f0 + 8, 1, :, :]
            h0 = Hv[:, f0:f0 + 8, 0, :, o].unsqueeze(3).to_broadcast([128, 8, 64, 2])
            h1 = Hv[:, f0:f0 + 8, 1, :, o].unsqueeze(3).to_broadcast([128, 8, 64, 2])
            z0 = Zv[:, f0:f0 + 8, 0, :, :]
            z1_ = Zv[:, f0:f0 + 8, 1, :, :]
            P.op("dve", lambda e, x0=x0, h0=h0: e.tensor_tensor(out=tv(m1), in0=x0, in1=h0, op=ALU.mult), reads=[X.b, Hs.b], writes=[m1.b])
            P.op("dve", lambda e, x1=x1, h1=h1: e.tensor_tensor(out=tv(m2), in0=x1, in1=h1, op=ALU.mult), reads=[X.b, Hs.b], writes=[m2.b])
            P.op("dve", lambda e, z0=z0: e.tensor_tensor(out=z0, in0=tv(m1), in1=tv(m2), op=ALU.subtract), reads=[m1.b, m2.b], writes=[Z.b])
            P.op("pool", lambda e, x0=x0, h1=h1: e.tensor_tensor(out=tv(n1), in0=x0, in1=h1, op=ALU.mult), reads=[X.b, Hs.b], writes=[n1.b])
            P.op("pool", lambda e, x1=x1, h0=h0: e.tensor_tensor(out=tv(n2), in0=x1, in1=h0, op=ALU.mult), reads=[X.b, Hs.b], writes=[n2.b])
            P.op("pool", lambda e, z1_=z1_: e.tensor_tensor(out=z1_, in0=tv(n1), in1=tv(n2), op=ALU.add), reads=[n1.b, n2.b], writes=[Z.b])

    def corner(V, W):
        Vv = V.t[:].rearrange("q (i c) -> q i c", c=128)
        for c0 in range(0, 128, 8):
            pw = pW[(c0 // 8) % 2]
            for j in range(8):
                P.op("pe", lambda e, pw=pw, j=j, c0=c0: e.transpose(out=pw.t[:, j * 128:(j + 1) * 128], in_=Vv[:, :, c0 + j], identity=ident.t[:]), reads=[V.b, ident.b], writes=[pw.b])
            evac(P, c0 // 8, W.t[:, c0 * 128:(c0 + 8) * 128], pw.t[:], [pw.b], [W.b])

    def stageAinv(W, o):
        Wv = W.t[:].rearrange("k (c b q) -> k c b q", b=2, q=128)
        gb = g0b if o == 0 else g1b
        for ch in range(16):
            pa = pA[cnt[0] % 4]
            cnt[0] += 1
            for b in range(2):
                P.op("pe", lambda e, pa=pa, b=b, ch=ch: e.matmul(pa.t[b * 64:(b + 1) * 64, :], lhsT=Einv.t[:], rhs=Wv[:, ch * 4:(ch + 1) * 4, b, :], start=True, stop=True), reads=[Einv.b, W.b], writes=[pa.b])
            sl = slice(ch * 512, (ch + 1) * 512)
            sk = skipb.t[:, o * 128 + ch * 8:o * 128 + ch * 8 + 8].unsqueeze(2).to_broadcast([128, 8, 64])
            P.op("dve", lambda e, sl=sl, sk=sk: e.tensor_tensor(out=m1.t[:, 0:512].rearrange("q (c p) -> q c p", p=64), in0=zb.t[:, sl].rearrange("q (c p) -> q c p", p=64), in1=sk, op=ALU.mult), reads=[zb.b, skipb.b], writes=[m1.b])
            P.op("dve", lambda e, pa=pa: e.tensor_tensor(out=m2.t[:, 0:512], in0=pa.t[:], in1=m1.t[:, 0:512], op=ALU.add), reads=[pa.b, m1.b], writes=[m2.b])
            P.op("dve", lambda e, sl=sl, gb=gb: e.tensor_tensor(out=zb.t[:, sl], in0=m2.t[:, 0:512], in1=gb.t[:, sl], op=ALU.mult), reads=[m2.b, gb.b], writes=[zb.b])

    stageA(tapsb, True)
    stageC(Hs)
    if dbg is not None:
        P.op("sp", lambda e: e.dma_start(out=dbg["Hs"][:, :], in_=Hs.t[:]), reads=[Hs.b], writes=[P.out_b], dma=True)
    for o in range(2):
        stageA(zb, False)
        if o == 0 and dbg is not None:
            P.op("sp", lambda e: e.dma_start(out=dbg["Y"][:, :], in_=bufA.t[:]), reads=[bufA.b], writes=[P.out_b], dma=True)
        stageC(bufB)
        if o == 0 and dbg is not None:
            P.op("sp", lambda e: e.dma_start(out=dbg["X"][:, :], in_=bufB.t[:]), reads=[bufB.b], writes=[P.out_b], dma=True)
        specmul(bufB, o, bufA)
        stageCinv(bufA, bufB)
        corner(bufB, bufA)
        stageAinv(bufA, o)
        if o == 0 and dbg is not None:
            P.op("sp", lambda e: e.dma_start(out=dbg["z1"][:, :], in_=zb.t[:]), reads=[zb.b], writes=[P.out_b], dma=True)
    zv = zb.t[:].rearrange("q (c p) -> q p c", p=64)
    for p0 in range(0, 64, 8):
        P.op("dve", lambda e, p0=p0: e.tensor_tensor(out=m1.t[:].rearrange("q (p c) -> q p c", p=8), in0=zv[:, p0:p0 + 8, :], in1=zv[:, p0:p0 + 8, :], op=ALU.mult), reads=[zb.b], writes=[m1.b])
        P.op("dve", lambda e, p0=p0: e.tensor_reduce(out=st.t[:, p0:p0 + 8], in_=m1.t[:].rearrange("q (p c) -> q p c", p=8), axis=AX.X, op=ALU.add), reads=[m1.b], writes=[st.b])
    P.op("act", lambda e: e.activation(out=st.t[:], in_=st.t[:], func=AF.Sqrt, scale=1.0 / 128, bias=EPS), reads=[st.b], writes=[st.b])
    P.op("dve", lambda e: e.reciprocal(out=st.t[:], in_=st.t[:]), reads=[st.b], writes=[st.b])
    P.op("dve", lambda e: e.tensor_tensor(out=g0b.t[:].rearrange("q (c p) -> q c p", p=64), in0=zb.t[:].rearrange("q (c p) -> q c p", p=64), in1=st.t[:].unsqueeze(1).to_broadcast([128, 128, 64]), op=ALU.mult), reads=[zb.b, st.b], writes=[g0b.b])
    hv = g0b.t[:].rearrange("q (c p) -> q c p", p=64)
    mv = g1b.t[:].rearrange("c (m p) -> c m p", p=64)
    for p0 in range(0, 64, 8):
        pw = pW[(p0 // 8) % 2]
        for j in range(8):
            P.op("pe", lambda e, pw=pw, j=j, p0=p0: e.transpose(out=pw.t[:, j * 128:(j + 1) * 128], in_=hv[:, :, p0 + j], identity=ident.t[:]), reads=[g0b.b, ident.b], writes=[pw.b])
        evac(P, p0 // 8, mv[:, :, p0:p0 + 8], pw.t[:].rearrange("c (p m) -> c m p", p=8), [pw.b], [g1b.b])
    P.op("sp", lambda e: e.dma_start(out=mixT[128:256, :], in_=g1b.t[:]), reads=[g1b.b], writes=[P.out_b], dma=True)

EPS = 1e-6


def build_l2():
    nc = bass.Bass("TRN2", target_bir_lowering=False)
    def din(name, shape, dt=F32): return nc.dram_tensor(name, list(shape), dt, kind="ExternalInput").ap()
    def dout(name, shape, dt=F32): return nc.dram_tensor(name, list(shape), dt, kind="ExternalOutput").ap()
    mixTc = din("mixTc", [2048, 1024], BF16); wout = din("wout", [2048, 2048]); gout = din("gout", [128, 16]); xc = din("xc", [1024, 2048])
    gffn = din("gffn", [128, 2048]); wr = din("wr", [2048, 16])
    x2o = dout("x2o", [1024, 2048]); ho = dout("ho", [1024, 2048], BF16); affo = dout("affo", [1024, 16])
    P = Prog(nc)
    Wo = P.tile("Wo", [128, 16 * 2048], BF16)
    wst = [P.tile("wst%d" % i, [128, 2048], F32) for i in range(2)]
    go = P.tile("go", [128, 16], F32)
    gf = P.tile("gf", [128, 2048], F32)
    wrf = P.tile("wrf", [128, 16 * 16], F32)
    wrb = P.tile("wrb", [128, 16 * 16], BF16)
    mT = P.tile("mT", [128, 16 * 1024], BF16)
    ident = P.tile("ident", [128, 128], BF16)
    identf = P.tile("identf", [128, 128], F32)
    xt = [P.tile("xt%d" % i, [128, 2048], F32) for i in range(2)]
    x2 = [P.tile("x2%d" % i, [128, 2048], F32) for i in range(2)]
    hb = [P.tile("hb%d" % i, [128, 2048], BF16) for i in range(2)]
    hT = P.tile("hT", [128, 2048], BF16)
    st = P.tile("st", [128, 64], F32)
    lg = P.tile("lg", [128, 16], F32)
    af = [P.tile("af%d" % i, [128, 16], F32) for i in range(2)]
    pacc = [P.tile("pacc%d" % i, [128, 512], F32, psum=True) for i in range(4)]
    pT = P.tile("pT", [128, 2048], BF16, psum=True)
    plog = P.tile("plog", [128, 512], F32, psum=True)
    P.op("pool", lambda e: e.memset(identf.t[:], 1.0), writes=[identf.b])
    P.op("pool", lambda e: e.affine_select(out=identf.t[:], in_=identf.t[:], pattern=[[-1, 128]], compare_op=ALU.is_equal, fill=0.0, base=0, channel_multiplier=1), reads=[identf.b], writes=[identf.b])
    P.op("dve", lambda e: e.tensor_copy(out=ident.t[:], in_=identf.t[:]), reads=[identf.b], writes=[ident.b])
    P.op("dve", lambda e: e.memset(st.t[:], 0.0), writes=[st.b])
    P.op("sp", lambda e: e.dma_start(out=go.t[:], in_=gout[:, :]), writes=[go.b], dma=True)
    P.op("sp", lambda e: e.dma_start(out=gf.t[:], in_=gffn[:, :]), writes=[gf.b], dma=True)
    P.op("sp", lambda e: e.dma_start(out=wrf.t[:].rearrange("p (k e) -> p k e", k=16), in_=wr.rearrange("(k p) e -> p k e", p=128)), writes=[wrf.b], dma=True)
    P.op("dve", lambda e: e.tensor_copy(out=wrb.t[:], in_=wrf.t[:]), reads=[wrf.b], writes=[wrb.b])
    P.op("sp", lambda e: e.dma_start(out=mT.t[:].rearrange("p (k t) -> p k t", k=16), in_=mixTc.rearrange("(k p) t -> p k t", p=128)), writes=[mT.b], dma=True)
    for dk in range(16):
        w = wst[dk % 2]
        P.op("sp", lambda e, w=w, dk=dk: e.dma_start(out=w.t[:], in_=wout[dk * 128:(dk + 1) * 128, :]), writes=[w.b], dma=True)
        P.op("act", lambda e, w=w, dk=dk: e.activation(out=Wo.t[:, dk * 2048:(dk + 1) * 2048], in_=w.t[:], func=AF.Copy, scale=go.t[:, dk:dk + 1]), reads=[w.b, go.b], writes=[Wo.b])
    mTv = mT.t[:].rearrange("p (k t) -> p k t", k=16)
    for tl in range(8):
        x_ = xt[tl % 2]; x2_ = x2[tl % 2]; hb_ = hb[tl % 2]; af_ = af[tl % 2]
        P.op("sp", lambda e, x_=x_, tl=tl: e.dma_start(out=x_.t[:], in_=xc[tl * 128:(tl + 1) * 128, :]), writes=[x_.b], dma=True)
        for nchunk in range(4):
            pa = pacc[nchunk]
            for dk in range(16):
                P.op("pe", lambda e, pa=pa, dk=dk, tl=tl, nchunk=nchunk: e.matmul(pa.t[:], lhsT=mTv[:, dk, tl * 128:(tl + 1) * 128], rhs=Wo.t[:, dk * 2048 + nchunk * 512:dk * 2048 + (nchunk + 1) * 512], start=(dk == 0), stop=(dk == 15)), reads=[mT.b, Wo.b], writes=[pa.b])
            P.op("dve", lambda e, pa=pa, x_=x_, x2_=x2_, nchunk=nchunk: e.tensor_tensor(out=x2_.t[:, nchunk * 512:(nchunk + 1) * 512], in0=pa.t[:], in1=x_.t[:, nchunk * 512:(nchunk + 1) * 512], op=ALU.add), reads=[pa.b, x_.b], writes=[x2_.b])
        P.op("sp", lambda e, x2_=x2_, tl=tl: e.dma_start(out=x2o[tl * 128:(tl + 1) * 128, :], in_=x2_.t[:]), reads=[x2_.b], writes=[P.out_b], dma=True)
        c = tl
        P.op("act", lambda e, x2_=x2_, hb_=hb_, c=c: e.activation(out=hb_.t[:], in_=x2_.t[:], func=AF.Square, accum_out=st.t[:, c:c + 1]), reads=[x2_.b], writes=[hb_.b, st.b])
        P.op("act", lambda e, c=c: e.activation(out=st.t[:, c:c + 1], in_=st.t[:, c:c + 1], func=AF.Sqrt, scale=1.0 / 2048, bias=EPS), reads=[st.b], writes=[st.b])
        P.op("dve", lambda e, c=c: e.reciprocal(out=st.t[:, c:c + 1], in_=st.t[:, c:c + 1]), reads=[st.b], writes=[st.b])
        P.op("dve", lambda e, x2_=x2_, hb_=hb_, c=c: e.scalar_tensor_tensor(out=hb_.t[:], in0=x2_.t[:], scalar=st.t[:, c:c + 1], in1=gf.t[:], op0=ALU.mult, op1=ALU.mult), reads=[x2_.b, st.b, gf.b], writes=[hb_.b])
        P.op("sp", lambda e, hb_=hb_, tl=tl: e.dma_start(out=ho[tl * 128:(tl + 1) * 128, :], in_=hb_.t[:]), reads=[hb_.b], writes=[P.out_b], dma=True)
        for dk in range(16):
            P.op("pe", lambda e, hb_=hb_, dk=dk: e.transpose(out=pT.t[:, dk * 128:(dk + 1) * 128], in_=hb_.t[:, dk * 128:(dk + 1) * 128], identity=ident.t[:]), reads=[hb_.b, ident.b], writes=[pT.b])
        P.op("act", lambda e: e.activation(out=hT.t[:], in_=pT.t[:], func=AF.Copy), reads=[pT.b], writes=[hT.b])
        for dk in range(16):
            P.op("pe", lambda e, dk=dk: e.matmul(plog.t[:, 0:16], lhsT=hT.t[:, dk * 128:(dk + 1) * 128], rhs=wrb.t[:, dk * 16:(dk + 1) * 16], start=(dk == 0), stop=(dk == 15)), reads=[hT.b, wrb.b], writes=[plog.b])
        c1 = 16 + tl; c2 = 32 + tl
        P.op("dve", lambda e, c1=c1: e.tensor_reduce(out=st.t[:, c1:c1 + 1], in_=plog.t[:, 0:16], axis=AX.X, op=ALU.max), reads=[plog.b], writes=[st.b])
        P.op("dve", lambda e, c1=c1: e.tensor_scalar(out=st.t[:, c1:c1 + 1], in0=st.t[:, c1:c1 + 1], scalar1=-1.0, scalar2=None, op0=ALU.mult), reads=[st.b], writes=[st.b])
        P.op("act", lambda e, c1=c1, c2=c2: e.activation(out=lg.t[:], in_=plog.t[:, 0:16], func=AF.Exp, bias=st.t[:, c1:c1 + 1], accum_out=st.t[:, c2:c2 + 1]), reads=[plog.b, st.b], writes=[lg.b, st.b])
        P.op("dve", lambda e, c2=c2: e.reciprocal(out=st.t[:, c2:c2 + 1], in_=st.t[:, c2:c2 + 1]), reads=[st.b], writes=[st.b])
        P.op("dve", lambda e, af_=af_, c2=c2: e.tensor_scalar(out=af_.t[:], in0=lg.t[:], scalar1=st.t[:, c2:c2 + 1], scalar2=None, op0=ALU.mult), reads=[lg.b, st.b], writes=[af_.b])
        P.op("sp", lambda e, af_=af_, tl=tl: e.dma_start(out=affo[tl * 128:(tl + 1) * 128, :], in_=af_.t[:]), reads=[af_.b], writes=[P.out_b], dma=True)
    P.finish([P.out_b])
    P.emit(); P.free_tiles()
    return nc


def l3_consts():
    k = np.arange(128)
    BO = (k[:, None] // 32 == k[None, :] // 32).astype(np.float32)
    SL = ((k[:, None] // 32 == k[None, :] // 32) & (k[:, None] % 32 < k[None, :] % 32)).astype(np.float32)
    U = (k[:, None] <= k[None, :]).astype(np.float32)
    iota = np.broadcast_to(np.arange(1, 513, dtype=np.float32)[None, :], (128, 512))
    col = np.arange(128)
    q = col // 32; th = col % 32; b = q % 2
    row = b[None, :] * 4096 + th[None, :] * 128 + k[:, None]
    rowhl = np.stack([row // 128, row % 128], axis=2).astype(np.float32)
    return dict(BO=BO, SL=SL, U=U, iota=np.ascontiguousarray(iota), rowhl=np.ascontiguousarray(rowhl.reshape(128, 256)))


def build_l3(NQ=4, NIT=32):
    nc = bass.Bass("TRN2", target_bir_lowering=False)
    def din(name, shape, dt=F32): return nc.dram_tensor(name, list(shape), dt, kind="ExternalInput").ap()
    def dout(name, shape, dt=F32): return nc.dram_tensor(name, list(shape), dt, kind="ExternalOutput").ap()
    affP = din("affP", [128, 128]); affTT = din("affTT", [128, 128]); BOd = din("BO", [128, 128]); SLd = din("SL", [128, 128]); Ud = din("U", [128, 128])
    iotad = din("iota", [128, 512]); rowhld = din("rowhl", [128, 256])
    h_all = din("h_all", [8192, 2048], BF16)
    wg = din("wg", [2, 2048, 1024]); wu = din("wu", [2, 2048, 1024]); wd = din("wd", [2, 1024, 2048])
    yeg = dout("yeg", [4, 512, 2048]); idxo = dout("idxo", [128, 16], I32)
    P = Prog(nc)
    ident = P.tile("ident", [128, 128], BF16); identf = P.tile("identf", [128, 128], F32)
    P.op("pool", lambda e: e.memset(identf.t[:], 1.0), writes=[identf.b])
    P.op("pool", lambda e: e.affine_select(out=identf.t[:], in_=identf.t[:], pattern=[[-1, 128]], compare_op=ALU.is_equal, fill=0.0, base=0, channel_multiplier=1), reads=[identf.b], writes=[identf.b])
    P.op("dve", lambda e: e.tensor_copy(out=ident.t[:], in_=identf.t[:]), reads=[identf.b], writes=[ident.b])
    Wg = P.tile("Wg", [128, 16 * 1024], BF16); Wu = P.tile("Wu", [128, 16 * 1024], BF16); Wd = P.tile("Wd", [128, 8 * 2048], BF16)
    idxi = P.tile("idxi", [128, 16], I32); gate = P.tile("gate", [128, 16], F32)
    mk0 = P.mark()
    aP = P.tile("aP", [128, 128], F32); aT = P.tile("aT", [128, 128], F32)
    BO = P.tile("BOt", [128, 128], F32); SLf = P.tile("SLf", [128, 128], F32); Uf = P.tile("Uf", [128, 128], F32)
    SLb = P.tile("SLb", [128, 128], BF16); Ub = P.tile("Ub", [128, 128], BF16)
    iota = P.tile("iotat", [128, 512], F32); rowhl = P.tile("rowhlt", [128, 256], F32)
    for t_, d_ in ((aP, affP), (aT, affTT), (BO, BOd), (SLf, SLd), (Uf, Ud), (iota, iotad), (rowhl, rowhld)):
        P.op("sp", lambda e, t_=t_, d_=d_: e.dma_start(out=t_.t[:], in_=d_[:, :]), writes=[t_.b], dma=True)
    P.op("dve", lambda e: e.tensor_copy(out=SLb.t[:], in_=SLf.t[:]), reads=[SLf.b], writes=[SLb.b])
    P.op("dve", lambda e: e.tensor_copy(out=Ub.t[:], in_=Uf.t[:]), reads=[Uf.b], writes=[Ub.b])
    lo = P.tile("lo", [128, 1], F32); hi = P.tile("hi", [128, 1], F32); mid = P.tile("mid", [128, 1], F32)
    cnt = P.tile("cnt", [128, 1], F32); pred = P.tile("pred", [128, 1], F32); d1 = P.tile("d1", [128, 1], F32)
    junk = P.tile("junk", [128, 128], F32)
    ptot = P.tile("ptot", [128, 512], F32, psum=True)
    P.op("dve", lambda e: e.memset(lo.t[:], 0.0), writes=[lo.b])
    P.op("dve", lambda e: e.memset(hi.t[:], 1.0), writes=[hi.b])
    for it in range(NIT):
        P.op("dve", lambda e: e.tensor_tensor(out=mid.t[:], in0=lo.t[:], in1=hi.t[:], op=ALU.add), reads=[lo.b, hi.b], writes=[mid.b])
        P.op("dve", lambda e: e.tensor_scalar(out=mid.t[:], in0=mid.t[:], scalar1=0.5, scalar2=None, op0=ALU.mult), reads=[mid.b], writes=[mid.b])
        P.op("dve", lambda e: e.tensor_scalar(out=junk.t[:], in0=aP.t[:], scalar1=mid.t[:, 0:1], scalar2=None, op0=ALU.is_gt), reads=[aP.b, mid.b], writes=[junk.b])
        P.op("dve", lambda e: e.tensor_reduce(out=cnt.t[:], in_=junk.t[:], axis=AX.X, op=ALU.add), reads=[junk.b], writes=[cnt.b])
        P.op("pe", lambda e: e.matmul(ptot.t[:, 0:1], lhsT=BO.t[:], rhs=cnt.t[:], start=True, stop=True), reads=[BO.b, cnt.b], writes=[ptot.b])
        P.op("dve", lambda e: e.tensor_scalar(out=pred.t[:], in0=ptot.t[:, 0:1], scalar1=511.5, scalar2=None, op0=ALU.is_gt), reads=[ptot.b], writes=[pred.b])
        P.op("dve", lambda e: e.tensor_tensor(out=d1.t[:], in0=mid.t[:], in1=lo.t[:], op=ALU.subtract), reads=[mid.b, lo.b], writes=[d1.b])
        P.op("dve", lambda e: e.scalar_tensor_tensor(out=lo.t[:], in0=d1.t[:], scalar=pred.t[:, 0:1], in1=lo.t[:], op0=ALU.mult, op1=ALU.add), reads=[d1.b, pred.b, lo.b], writes=[lo.b])
        P.op("dve", lambda e: e.tensor_tensor(out=d1.t[:], in0=hi.t[:], in1=mid.t[:], op=ALU.subtract), reads=[mid.b, hi.b], writes=[d1.b])
        P.op("dve", lambda e: e.scalar_tensor_tensor(out=hi.t[:], in0=d1.t[:], scalar=pred.t[:, 0:1], in1=mid.t[:], op0=ALU.mult, op1=ALU.add), reads=[d1.b, pred.b, mid.b], writes=[hi.b])
    maskb = P.tile("maskb", [128, 128], BF16); maskT = P.tile("maskT", [128, 128], F32); maskTb = P.tile("maskTb", [128, 128], BF16)
    cntB = P.tile("cntB", [128, 128], BF16); slotI = P.tile("slotI", [128, 128], F32)
    pmT = P.tile("pmT", [128, 1024], BF16, psum=True)
    ppf = P.tile("ppf", [128, 512], F32, psum=True)
    P.op("dve", lambda e: e.tensor_scalar(out=junk.t[:], in0=aP.t[:], scalar1=lo.t[:, 0:1], scalar2=None, op0=ALU.is_gt), reads=[aP.b, lo.b], writes=[junk.b])
    P.op("dve", lambda e: e.tensor_reduce(out=cnt.t[:], in_=junk.t[:], axis=AX.X, op=ALU.add), reads=[junk.b], writes=[cnt.b])
    P.op("dve", lambda e: e.tensor_copy(out=maskb.t[:], in_=junk.t[:]), reads=[junk.b], writes=[maskb.b])
    P.op("pe", lambda e: e.transpose(out=pmT.t[:, 0:128], in_=maskb.t[:], identity=ident.t[:]), reads=[maskb.b, ident.b], writes=[pmT.b])
    P.op("dve", lambda e: e.tensor_copy(out=maskT.t[:], in_=pmT.t[:, 0:128]), reads=[pmT.b], writes=[maskT.b])
    P.op("dve", lambda e: e.tensor_copy(out=maskTb.t[:], in_=pmT.t[:, 0:128]), reads=[pmT.b], writes=[maskTb.b])
    P.op("dve", lambda e: e.tensor_scalar(out=cntB.t[:], in0=identf.t[:], scalar1=0.0, scalar2=cnt.t[:, 0:1], op0=ALU.mult, op1=ALU.add), reads=[identf.b, cnt.b], writes=[cntB.b])
    P.op("pe", lambda e: e.matmul(ppf.t[:, 0:128], lhsT=Ub.t[:], rhs=maskTb.t[:], start=True, stop=False), reads=[Ub.b, maskTb.b], writes=[ppf.b])
    P.op("pe", lambda e: e.matmul(ppf.t[:, 0:128], lhsT=cntB.t[:], rhs=SLb.t[:], start=False, stop=True), reads=[cntB.b, SLb.b], writes=[ppf.b])
    P.op("dve", lambda e: e.tensor_copy(out=slotI.t[:], in_=ppf.t[:, 0:128]), reads=[ppf.b], writes=[slotI.b])
    val = P.tile("val", [128, 128 * 3], F32)
    vv = val.t[:].rearrange("p (c k) -> p c k", k=3)
    P.op("dve", lambda e: e.tensor_copy(out=vv[:, :, 0:2], in_=rowhl.t[:].rearrange("p (c k) -> p c k", k=2)), reads=[rowhl.b], writes=[val.b])
    P.op("dve", lambda e: e.tensor_copy(out=vv[:, :, 2:3], in_=aT.t[:].unsqueeze(2)), reads=[aT.b], writes=[val.b])
    oh = [P.tile("oh%d" % i, [128, 512], F32) for i in range(2)]
    pidx = [P.tile("pidx%d" % i, [128, 512], F32, psum=True) for i in range(4)]
    res = P.tile("res", [128, 16 * 3], F32)
    for q in range(NQ):
        for th in range(32):
            col = q * 32 + th
            o_ = oh[th % 2]
            P.op("dve", lambda e, o_=o_, col=col: e.tensor_scalar(out=o_.t[:], in0=iota.t[:], scalar1=slotI.t[:, col:col + 1], scalar2=maskT.t[:, col:col + 1], op0=ALU.is_equal, op1=ALU.mult), reads=[iota.b, slotI.b, maskT.b], writes=[o_.b])
            for sc in range(4):
                pi = pidx[sc]
                P.op("pe", lambda e, pi=pi, o_=o_, sc=sc, col=col, th=th: e.matmul(pi.t[:, 0:3], lhsT=o_.t[:, sc * 128:(sc + 1) * 128], rhs=vv[:, col, :], start=(th == 0), stop=(th == 31)), reads=[o_.b, val.b], writes=[pi.b])
        for sc in range(4):
            pi = pidx[sc]
            P.op("dve", lambda e, pi=pi, q=q, sc=sc: e.tensor_copy(out=res.t[:, (q * 4 + sc) * 3:(q * 4 + sc) * 3 + 3], in_=pi.t[:, 0:3]), reads=[pi.b], writes=[res.b])
    rv = res.t[:].rearrange("p (c k) -> p c k", k=3)
    idxf = P.tile("idxf", [128, 16], F32)
    P.op("dve", lambda e: e.scalar_tensor_tensor(out=idxf.t[:].unsqueeze(2), in0=rv[:, :, 0:1], scalar=128.0, in1=rv[:, :, 1:2], op0=ALU.mult, op1=ALU.add), reads=[res.b], writes=[idxf.b])
    P.op("dve", lambda e: e.tensor_copy(out=idxi.t[:], in_=idxf.t[:]), reads=[idxf.b], writes=[idxi.b])
    P.op("dve", lambda e: e.tensor_copy(out=gate.t[:].unsqueeze(2), in_=rv[:, :, 2:3]), reads=[res.b], writes=[gate.b])
    P.op("sp", lambda e: e.dma_start(out=idxo[:, :], in_=idxi.t[:]), reads=[idxi.b], writes=[P.out_b], dma=True)
    P.barrier()
    P.free_to(mk0)
    xg = [P.tile("xg%d" % i, [128, 2048], BF16) for i in range(2)]
    xgT = P.tile("xgT", [128, 16 * 512], BF16)
    actT = P.tile("actT", [128, 8 * 512], BF16)
    sg = [P.tile("sg%d" % i, [128, 512], F32) for i in range(2)]
    yt = [P.tile("yt%d" % i, [128, 2048], F32) for i in range(2)]
    pT = P.tile("pTg", [128, 2048], BF16, psum=True)
    pg = [P.tile("pg%d" % i, [128, 512], F32, psum=True) for i in range(2)]
    pu = [P.tile("pu%d" % i, [128, 512], F32, psum=True) for i in range(2)]
    pd = [P.tile("pd%d" % i, [128, 512], F32, psum=True) for i in range(2)]
    xgTv = xgT.t[:].rearrange("p (k s) -> p k s", k=16)
    n = 0
    for q in range(NQ):
        el = q // 2
        if q % 2 == 0:
            for part in range(4):
                P.op("pool", lambda e, el=el, part=part: e.dma_start(out=Wg.t[:, part * 4096:(part + 1) * 4096].rearrange("p (k f) -> p k f", k=4), in_=wg[el, part * 512:(part + 1) * 512, :].rearrange("(k p) f -> p k f", p=128)), writes=[Wg.b], dma=True)
                P.op("pool", lambda e, el=el, part=part: e.dma_start(out=Wu.t[:, part * 4096:(part + 1) * 4096].rearrange("p (k f) -> p k f", k=4), in_=wu[el, part * 512:(part + 1) * 512, :].rearrange("(k p) f -> p k f", p=128)), writes=[Wu.b], dma=True)
                P.op("pool", lambda e, el=el, part=part: e.dma_start(out=Wd.t[:, part * 4096:(part + 1) * 4096].rearrange("p (k f) -> p k f", k=2), in_=wd[el, part * 256:(part + 1) * 256, :].rearrange("(k p) f -> p k f", p=128)), writes=[Wd.b], dma=True)
        for sc in range(4):
            x_ = xg[sc % 2]
            c = q * 4 + sc
            P.op("pool", lambda e, x_=x_, c=c: e.indirect_dma_start(out=x_.t[:, :], out_offset=None, in_=h_all[:, :], in_offset=bass.IndirectOffsetOnAxis(ap=idxi.t[:, c:c + 1], axis=0)), reads=[idxi.b], writes=[x_.b], dma=True)
            for dk in range(16):
                P.op("pe", lambda e, x_=x_, dk=dk: e.transpose(out=pT.t[:, dk * 128:(dk + 1) * 128], in_=x_.t[:, dk * 128:(dk + 1) * 128], identity=ident.t[:]), reads=[x_.b, ident.b], writes=[pT.b])
            evac(P, sc, xgTv[:, :, sc * 128:(sc + 1) * 128], pT.t[:].rearrange("p (k s) -> p k s", k=16), [pT.b], [xgT.b])
        for fc in range(8):
            pg_ = pg[fc % 2]; pu_ = pu[fc % 2]; sg_ = sg[fc % 2]
            for dk in range(16):
                P.op("pe", lambda e, pg_=pg_, dk=dk, fc=fc: e.matmul(pg_.t[:], lhsT=Wg.t[:, dk * 1024 + fc * 128:dk * 1024 + (fc + 1) * 128], rhs=xgTv[:, dk, :], start=(dk == 0), stop=(dk == 15)), reads=[Wg.b, xgT.b], writes=[pg_.b])
            for dk in range(16):
                P.op("pe", lambda e, pu_=pu_, dk=dk, fc=fc: e.matmul(pu_.t[:], lhsT=Wu.t[:, dk * 1024 + fc * 128:dk * 1024 + (fc + 1) * 128], rhs=xgTv[:, dk, :], start=(dk == 0), stop=(dk == 15)), reads=[Wu.b, xgT.b], writes=[pu_.b])
            P.op("act", lambda e, pg_=pg_, sg_=sg_: e.activation(out=sg_.t[:], in_=pg_.t[:], func=AF.Silu), reads=[pg_.b], writes=[sg_.b])
            P.op("dve", lambda e, pu_=pu_, sg_=sg_, fc=fc: e.tensor_tensor(out=actT.t[:, fc * 512:(fc + 1) * 512], in0=sg_.t[:], in1=pu_.t[:], op=ALU.mult), reads=[sg_.b, pu_.b], writes=[actT.b])
        for sc in range(4):
            y_ = yt[sc % 2]
            c = q * 4 + sc
            for dc in range(4):
                pd_ = pd[dc % 2]
                for fc in range(8):
                    P.op("pe", lambda e, pd_=pd_, fc=fc, sc=sc, dc=dc: e.matmul(pd_.t[:], lhsT=actT.t[:, fc * 512 + sc * 128:fc * 512 + (sc + 1) * 128], rhs=Wd.t[:, fc * 2048 + dc * 512:fc * 2048 + (dc + 1) * 512], start=(fc == 0), stop=(fc == 7)), reads=[actT.b, Wd.b], writes=[pd_.b])
                P.op("act", lambda e, pd_=pd_, y_=y_, dc=dc, c=c: e.activation(out=y_.t[:, dc * 512:(dc + 1) * 512], in_=pd_.t[:], func=AF.Copy, scale=gate.t[:, c:c + 1]), reads=[pd_.b, gate.b], writes=[y_.b])
            P.op("sp", lambda e, y_=y_, q=q, sc=sc: e.dma_start(out=yeg[q, sc * 128:(sc + 1) * 128, :], in_=y_.t[:]), reads=[y_.b], writes=[P.out_b], dma=True)
    P.finish([P.out_b])
    P.emit(); P.free_tiles()
    return nc


def evac(P, i, out, in_, reads, writes):
    if i % 2 == 0:
        P.op("act", lambda e: e.activation(out=out, in_=in_, func=AF.Copy), reads=reads, writes=writes)
    else:
        P.op("dve", lambda e: e.tensor_copy(out=out, in_=in_), reads=reads, writes=writes)

def build_l4():
    nc = bass.Bass("TRN2", target_bir_lowering=False)
    def din(name, shape, dt=F32): return nc.dram_tensor(name, list(shape), dt, kind="ExternalInput").ap()
    x2c = din("x2c", [1024, 2048]); yegb = din("yegb", [8192, 2048]); idxb = din("idxb", [128, 64], I32); basev = din("basev", [128, 1])
    out = nc.dram_tensor("out", [1024, 2048], F32, kind="ExternalOutput").ap()
    P = Prog(nc)
    idi = P.tile("idi", [128, 64], I32); idf = P.tile("idf", [128, 64], F32); v1 = P.tile("v1", [128, 64], F32); v2 = P.tile("v2", [128, 64], F32)
    loci = P.tile("loci", [128, 64], I32); bs = P.tile("bs", [128, 1], F32)
    yt = [P.tile("y%d" % i, [128, 2048], F32) for i in range(3)]
    P.op("sp", lambda e: e.dma_start(out=idi.t[:], in_=idxb[:, :]), writes=[idi.b], dma=True)
    P.op("sp", lambda e: e.dma_start(out=bs.t[:], in_=basev[:, :]), writes=[bs.b], dma=True)
    P.op("sp", lambda e: e.dma_start(out=out[:, :], in_=x2c[:, :]), writes=[P.out_b], dma=True)
    P.op("dve", lambda e: e.tensor_copy(out=idf.t[:], in_=idi.t[:]), reads=[idi.b], writes=[idf.b])
    P.op("dve", lambda e: e.tensor_scalar(out=idf.t[:], in0=idf.t[:], scalar1=bs.t[:, 0:1], scalar2=None, op0=ALU.subtract), reads=[idf.b, bs.b], writes=[idf.b])
    P.op("dve", lambda e: e.tensor_scalar(out=v1.t[:], in0=idf.t[:], scalar1=-0.5, scalar2=None, op0=ALU.is_gt), reads=[idf.b], writes=[v1.b])
    P.op("dve", lambda e: e.tensor_scalar(out=v2.t[:], in0=idf.t[:], scalar1=1023.5, scalar2=None, op0=ALU.is_lt), reads=[idf.b], writes=[v2.b])
    P.op("dve", lambda e: e.tensor_tensor(out=v1.t[:], in0=v1.t[:], in1=v2.t[:], op=ALU.mult), reads=[v1.b, v2.b], writes=[v1.b])
    P.op("dve", lambda e: e.tensor_scalar(out=idf.t[:], in0=idf.t[:], scalar1=-4096.0, scalar2=None, op0=ALU.add), reads=[idf.b], writes=[idf.b])
    P.op("dve", lambda e: e.tensor_tensor(out=idf.t[:], in0=idf.t[:], in1=v1.t[:], op=ALU.mult), reads=[idf.b, v1.b], writes=[idf.b])
    P.op("dve", lambda e: e.tensor_scalar(out=idf.t[:], in0=idf.t[:], scalar1=4096.0, scalar2=None, op0=ALU.add), reads=[idf.b], writes=[idf.b])
    P.op("dve", lambda e: e.tensor_copy(out=loci.t[:], in_=idf.t[:]), reads=[idf.b], writes=[loci.b])
    _bc = {}

    def BC(e):
        if "v" not in _bc:
            r = e.alloc_register("bc")
            e.reg_mov(r, 1023)
            _bc["v"] = r
        return _bc["v"]

    for col in range(64):
        y_ = yt[col % 3]
        P.op("sp", lambda e, y_=y_, col=col: e.dma_start(out=y_.t[:], in_=yegb[col * 128:(col + 1) * 128, :]), writes=[y_.b], dma=True)
        P.op("pool", lambda e, y_=y_, col=col: e.indirect_dma_start(out=out[:, :], out_offset=bass.IndirectOffsetOnAxis(ap=loci.t[:, col:col + 1], axis=0), in_=y_.t[:, :], in_offset=None, bounds_check=BC(e), oob_is_err=False, compute_op=ALU.add), reads=[y_.b, loci.b, P.out_b], writes=[P.out_b], dma=True)
    P.finish([P.out_b])
    P.emit(); P.free_tiles()
    return nc

def build_l1():
    nc = bass.Bass("TRN2", target_bir_lowering=False)
    def din(name, shape, dt=F32): return nc.dram_tensor(name, list(shape), dt, kind="ExternalInput").ap()
    x = din("x", [8192, 2048]); watt = din("watt", [2048, 384]); why = din("why", [2048, 384]); gmix = din("gmix", [128, 16]); gqk = din("gqk", [128, 2])
    bt = din("bt", [128, 3200]); mk = din("mk", [128, 3200])
    D = dict(cwb=din("cwb", [128, 12]), zposT=din("zposT", [33, 8192]), w1=din("w1", [33, 64]), w2=din("w2", [64, 64]), w3h=din("w3h", [64, 512]), fv=din("fv", [64, 3]),
             winsg=din("winsg", [128, 8192]), Fblk=din("Fblk", [128, 256], BF16), F128=din("F128", [128, 128], BF16), Gf=din("Gf", [128, 64 * 384], BF16), Gi=din("Gi", [128, 64 * 384], BF16),
             Einv=din("Einv", [128, 64], BF16), skipb=din("skipb", [128, 256]))
    mixT = nc.dram_tensor("mixT", [256, 8192], BF16, kind="ExternalOutput").ap()
    D["rawD"] = nc.dram_tensor("rawD", [3, 128, 8192], F32, kind="Internal").ap()
    P = Prog(nc)
    phase_proj_attn(nc, P, x, watt, gmix, gqk, bt, mk, mixT, None, NBLK=16, why=why, rawD=D["rawD"])
    P.barrier(); P.free_tiles()
    phase_hyena(nc, P, D, mixT, dbg=None, stage="all")
    P.finish([P.out_b, P.xTs_b])
    P.emit(); P.free_tiles()
    return nc


def kernel(x, mix_norm_g, w_in, q_norm_g, k_norm_g, rpb, conv_w, conv_b, filt_w1, filt_b1, filt_w2, filt_b2, filt_w3, filt_freq, filt_bias,
           attn_out_g, hyena_out_g, w_out, ffn_norm_g, w_router, w_gate, w_up, w_down):
    A = lambda v: np.ascontiguousarray(np.asarray(v))
    x = np.asarray(x, np.float32); xf = x.reshape(8192, 2048)
    w_in0 = np.asarray(w_in)[0]
    cores = list(range(8))
    fc = fft_consts()
    gmix = A(np.asarray(mix_norm_g)[0].reshape(16, 128).T)
    gqk = A(np.stack([np.asarray(q_norm_g)[0], np.asarray(k_norm_g)[0]], axis=1))
    cw = np.asarray(conv_w)[0]; cbias = np.asarray(conv_b)[0]
    fv = A(np.stack([np.asarray(filt_freq)[0], np.asarray(filt_b1)[0], np.asarray(filt_b2)[0]], axis=1))
    in1 = []
    for h in cores:
        c0 = 128 * h
        watt = np.concatenate([w_in0[:, c0:c0 + 128], w_in0[:, 1024 + c0:1024 + c0 + 128], w_in0[:, 2048 + c0:2048 + c0 + 128]], axis=1)
        why = np.concatenate([w_in0[:, 3072 + c0:3072 + c0 + 128], w_in0[:, 4096 + c0:4096 + c0 + 128], w_in0[:, 5120 + c0:5120 + c0 + 128]], axis=1)
        BT, MK = att_tables(np.asarray(rpb)[0, h])
        cwb = np.zeros((128, 12), np.float32)
        for s in range(3):
            for j in range(3):
                cwb[:, 3 * s + j] = cw[j, s * 1024 + c0:s * 1024 + c0 + 128]
            cwb[:, 9 + s] = cbias[s * 1024 + c0:s * 1024 + c0 + 128]
        zposT, winsg = pos_consts(h)
        w3 = np.asarray(filt_w3)[0].reshape(64, 2, 2, 1024)[:, :, :, c0:c0 + 128].reshape(64, 512)
        skipb = np.broadcast_to(np.asarray(filt_bias)[0][:, c0:c0 + 128].reshape(1, 256), (128, 256))
        m = {"x": xf, "watt": watt, "why": why, "gmix": gmix, "gqk": gqk, "bt": BT, "mk": MK, "cwb": cwb, "zposT": zposT, "w1": np.asarray(filt_w1)[0], "w2": np.asarray(filt_w2)[0],
             "w3h": w3, "fv": fv, "winsg": winsg, "skipb": skipb, **fc}
        in1.append({k: A(v) for k, v in m.items()})
    r1 = run_bass_kernel_spmd(build_l1(), in1, core_ids=cores).results
    mixed_T = np.concatenate([np.asarray(r1[h]["mixT"])[0:128] for h in cores] + [np.asarray(r1[h]["mixT"])[128:256] for h in cores], axis=0)
    gout = np.concatenate([np.asarray(attn_out_g)[0], np.asarray(hyena_out_g)[0]])
    goutl = A(gout.reshape(16, 128).T)
    gffn = A(np.broadcast_to(np.asarray(ffn_norm_g)[0][None, :], (128, 2048)))
    in2 = []
    for c in cores:
        tok = slice(1024 * c, 1024 * c + 1024)
        in2.append({"mixTc": A(mixed_T[:, tok]), "wout": A(np.asarray(w_out)[0]), "gout": goutl, "xc": A(xf[tok]), "gffn": gffn, "wr": A(np.asarray(w_router)[0])})
    r2 = run_bass_kernel_spmd(build_l2(), in2, core_ids=cores).results
    x2 = [np.asarray(r2[c]["x2o"]) for c in cores]
    h_all = A(np.concatenate([np.asarray(r2[c]["ho"]) for c in cores], axis=0))
    aff = np.concatenate([np.asarray(r2[c]["affo"]) for c in cores], axis=0).reshape(2, 4096, 16)
    cst = l3_consts()
    probs = [(el, b) for el in range(2) for b in range(2)]
    in3 = []
    for j in cores:
        affq = np.stack([aff[b, :, 2 * j + el] for el, b in probs])
        in3.append({"affP": A(affq.reshape(128, 128)), "affTT": A(affq.reshape(4, 32, 128).transpose(2, 0, 1).reshape(128, 128)), **cst, "h_all": h_all,
                    "wg": A(np.asarray(w_gate)[0, 2 * j:2 * j + 2]), "wu": A(np.asarray(w_up)[0, 2 * j:2 * j + 2]), "wd": A(np.asarray(w_down)[0, 2 * j:2 * j + 2])})
    r3 = run_bass_kernel_spmd(build_l3(), in3, core_ids=cores).results
    in4 = []
    for c in cores:
        b = c // 4
        ys = []; ids = []
        for e in range(16):
            j = e // 2; q = (e % 2) * 2 + b
            ys.append(np.asarray(r3[j]["yeg"])[q]); ids.append(np.asarray(r3[j]["idxo"])[:, q * 4:(q + 1) * 4])
        in4.append({"x2c": A(x2[c]), "yegb": A(np.concatenate(ys, axis=0)), "idxb": A(np.concatenate(ids, axis=1).astype(np.int32)), "basev": np.full((128, 1), 1024.0 * c, np.float32)})
    r4 = run_bass_kernel_spmd(build_l4(), in4, core_ids=cores).results
    out = np.concatenate([np.asarray(r4[c]["out"]) for c in cores], axis=0).reshape(2, 4096, 2048).astype(np.float32)
    return out
```
